# Optimizing a Trainium2 kernel written in Bass

```python
import jax, jax.numpy as jnp
from jax import lax
import numpy as np

D_MODEL = 1024
BATCH = 32
SEQ = 2048
DEPTH = 1

POOL_WIDTH = D_MODEL // 2
POOL_GROUPS = 4
POOL_GROUP_DIM = POOL_WIDTH // POOL_GROUPS
POOL_WINDOWS = (2, 4, 8, 16)
MAX_WINDOW = max(POOL_WINDOWS)
CONV_WIDTH = D_MODEL // 2
CONV_KERNEL = 31
N_BRANCHES = 2
IN_PROJ_DIM = POOL_WIDTH + 2 * CONV_WIDTH + N_BRANCHES * D_MODEL
PEER_HEADS = 8
PEER_N_KEYS = 128
PEER_N_EXPERTS = PEER_N_KEYS * PEER_N_KEYS
PEER_TOPK = 16
PEER_KEY_DIM = 256
PEER_HALF = PEER_KEY_DIM // 2
PEER_TOKEN_BLOCK = 128
EPS = 1e-6

kernel_name = "gated_pool_conformer_peer_block"


def rmsnorm(x, g):
    xf = x.astype(jnp.float32)
    y = xf * lax.rsqrt(jnp.mean(xf * xf, axis=-1, keepdims=True) + EPS)
    return (y * g.astype(jnp.float32)).astype(x.dtype)


def layernorm(x, g, b):
    xf = x.astype(jnp.float32)
    mu = jnp.mean(xf, axis=-1, keepdims=True)
    var = jnp.mean(jnp.square(xf - mu), axis=-1, keepdims=True)
    y = (xf - mu) * lax.rsqrt(var + EPS)
    return (y * g.astype(jnp.float32) + b.astype(jnp.float32)).astype(x.dtype)


def causal_multiscale_pool(z):
    B, S, _ = z.shape
    zg = z.reshape(B, S, POOL_GROUPS, POOL_GROUP_DIM).astype(jnp.float32)
    c = jnp.cumsum(zg, axis=1)
    cpad = jnp.pad(c, ((0, 0), (MAX_WINDOW, 0), (0, 0), (0, 0)))
    pos = jnp.arange(S, dtype=jnp.float32)
    outs = []
    for gi, w in enumerate(POOL_WINDOWS):
        window_sum = c[:, :, gi] - cpad[:, MAX_WINDOW - w:MAX_WINDOW - w + S, gi]
        count = jnp.minimum(pos + 1.0, float(w))[None, :, None]
        outs.append(window_sum / count)
    pooled = jnp.stack(outs, axis=2)
    return (pooled - zg).astype(z.dtype)


def causal_depthwise_conv(u, w, b):
    C = u.shape[-1]
    k = w.reshape(CONV_KERNEL, 1, C).astype(u.dtype)
    y = lax.conv_general_dilated(u, k, window_strides=(1,), padding=[(CONV_KERNEL - 1, 0)],
                                 dimension_numbers=('NWC', 'WIO', 'NWC'), feature_group_count=C)
    return y + b.astype(u.dtype)


def peer_token_block(hb, w_q, sub_keys, expert_u, expert_v):
    T = hb.shape[0]
    q = (hb @ w_q).reshape(T, PEER_HEADS, 2, PEER_HALF)
    s = jnp.einsum('thpc,hpkc->thpk', q, sub_keys).astype(jnp.float32)
    vals, idx = lax.top_k(s, PEER_TOPK)
    cand = vals[:, :, 0, :, None] + vals[:, :, 1, None, :]
    cand = cand.reshape(T, PEER_HEADS, PEER_TOPK * PEER_TOPK)
    best, pos = lax.top_k(cand, PEER_TOPK)
    i1 = jnp.take_along_axis(idx[:, :, 0], pos // PEER_TOPK, axis=-1)
    i2 = jnp.take_along_axis(idx[:, :, 1], pos % PEER_TOPK, axis=-1)
    expert = i1 * PEER_N_KEYS + i2
    gate = jax.nn.softmax(best, axis=-1)
    u = jnp.take(expert_u, expert, axis=0)
    v = jnp.take(expert_v, expert, axis=0)
    act = jax.nn.gelu(jnp.einsum('thkd,td->thk', u, hb))
    return jnp.einsum('thk,thkd->td', (gate.astype(hb.dtype) * act), v)


def setup_inputs(seed: int = 0) -> dict:
    key = jax.random.key(seed)
    ks = jax.random.split(key, 24)
    f32 = jnp.float32
    L, D = DEPTH, D_MODEL
    nrm = lambda k, shape, scale: jax.random.normal(k, shape, f32) * scale
    return {
        "x": jax.random.normal(ks[0], (BATCH, SEQ, D), f32),
        "mix_norm_g": 1.0 + nrm(ks[1], (L, D), 0.02),
        "w_in": nrm(ks[2], (L, D, IN_PROJ_DIM), D ** -0.5),
        "pool_w": nrm(ks[3], (L, POOL_GROUPS, POOL_GROUP_DIM, POOL_GROUP_DIM), POOL_GROUP_DIM ** -0.5),
        "pool_scale": 1.0 + nrm(ks[4], (L, POOL_WIDTH), 0.02),
        "w_branch_a": nrm(ks[5], (L, POOL_WIDTH, D), POOL_WIDTH ** -0.5),
        "conv_w": nrm(ks[6], (L, CONV_KERNEL, CONV_WIDTH), CONV_KERNEL ** -0.5),
        "conv_b": nrm(ks[7], (L, CONV_WIDTH), 0.02),
        "conv_ln_g": 1.0 + nrm(ks[8], (L, CONV_WIDTH), 0.02),
        "conv_ln_b": nrm(ks[9], (L, CONV_WIDTH), 0.02),
        "w_branch_b": nrm(ks[10], (L, CONV_WIDTH, D), CONV_WIDTH ** -0.5),
        "gate_b": nrm(ks[11], (L, N_BRANCHES, D), 0.02),
        "w_out": nrm(ks[12], (L, D, D), D ** -0.5),
        "ffn_norm_g": 1.0 + nrm(ks[13], (L, D), 0.02),
        "peer_w_q": nrm(ks[14], (L, D, PEER_HEADS * PEER_KEY_DIM), D ** -0.5),
        "peer_sub_keys": nrm(ks[15], (L, PEER_HEADS, 2, PEER_N_KEYS, PEER_HALF), PEER_HALF ** -0.5),
        "peer_u": nrm(ks[16], (L, PEER_N_EXPERTS, D), D ** -0.5),
        "peer_v": nrm(ks[17], (L, PEER_N_EXPERTS, D), PEER_HEADS ** -0.5),
        "final_norm_g": 1.0 + nrm(ks[18], (D,), 0.02),
    }


def reference(x, mix_norm_g, w_in, pool_w, pool_scale, w_branch_a, conv_w, conv_b, conv_ln_g,
              conv_ln_b, w_branch_b, gate_b, w_out, ffn_norm_g, peer_w_q, peer_sub_keys,
              peer_u, peer_v, final_norm_g):
    B, S, D = x.shape
    T = B * S
    for l in range(DEPTH):
        h = rmsnorm(x, mix_norm_g[l])
        proj = h @ w_in[l]
        za = proj[..., :POOL_WIDTH]
        zb = proj[..., POOL_WIDTH:POOL_WIDTH + 2 * CONV_WIDTH]
        zg = proj[..., POOL_WIDTH + 2 * CONV_WIDTH:].reshape(B, S, N_BRANCHES, D)
        pa = causal_multiscale_pool(za)
        pa = jnp.einsum('bsgc,gcd->bsgd', pa, pool_w[l]).reshape(B, S, POOL_WIDTH) * pool_scale[l]
        ya = pa @ w_branch_a[l]
        ub = jax.nn.glu(zb, axis=-1)
        ub = causal_depthwise_conv(ub, conv_w[l], conv_b[l])
        ub = jax.nn.swish(layernorm(ub, conv_ln_g[l], conv_ln_b[l]))
        yb = ub @ w_branch_b[l]
        gates = jax.nn.sigmoid(zg + gate_b[l])
        merged = gates[:, :, 0] * ya + gates[:, :, 1] * yb
        x = x + merged @ w_out[l]
        h = rmsnorm(x, ffn_norm_g[l])
        hb = h.reshape(T // PEER_TOKEN_BLOCK, PEER_TOKEN_BLOCK, D)
        wq_l, sk_l, u_l, v_l = peer_w_q[l], peer_sub_keys[l], peer_u[l], peer_v[l]
        y = lax.map(lambda blk: peer_token_block(blk, wq_l, sk_l, u_l, v_l), hb)
        x = x + y.reshape(B, S, D)
    return rmsnorm(x, final_norm_g)
```

```python
import numpy as np
import concourse.bass as bass
import concourse.mybir as mybir
from concourse.bass_utils import run_bass_kernel_spmd

F32 = mybir.dt.float32
BF16 = mybir.dt.bfloat16
I32 = mybir.dt.int32
U32 = mybir.dt.uint32
ALU = mybir.AluOpType
AF = mybir.ActivationFunctionType
AX = mybir.AxisListType

D = 1024
NCORES = 8
EPS = 1e-6
NEG = -1.0e30


class Buf:
    __slots__ = ("name", "w", "r")

    def __init__(self, name):
        self.name = name
        self.w = None
        self.r = {}


class Sync:
    ENG = ("pe", "act", "dve", "pool", "sp")

    def __init__(self, nc, n_dma_sp=8, n_dma_pool=24, n_dma_act=2):
        self.nc = nc
        self.semh = {}
        self.cnt = {}
        self.prog = {e: [] for e in self.ENG}
        self.seen = {e: {} for e in self.ENG}
        for e in self.ENG:
            self.semh[e] = nc.alloc_semaphore(name="s_" + e)
            self.cnt[e] = 0
        self.dq = {}
        for q, n in (("sp", n_dma_sp), ("pool", n_dma_pool), ("act", n_dma_act)):
            keys = []
            for i in range(n):
                k = ("dma", q, i)
                self.semh[k] = nc.alloc_semaphore(name="d_%s_%d" % (q, i))
                self.cnt[k] = 0
                keys.append(k)
            self.dq[q] = [keys, 0]
        self.n_inst = 0
        self.n_wait = 0

    def _deps(self, eng, reads, writes):
        need = {}
        for b in reads:
            if b.w is not None and need.get(b.w[0], 0) < b.w[1]:
                need[b.w[0]] = b.w[1]
        for b in writes:
            if b.w is not None and need.get(b.w[0], 0) < b.w[1]:
                need[b.w[0]] = b.w[1]
            for k, v in b.r.items():
                if need.get(k, 0) < v:
                    need[k] = v
        seen = self.seen[eng]
        out = []
        for k, v in need.items():
            if eng == "pe" and k == "pe":
                continue
            if seen.get(k, 0) >= v:
                continue
            seen[k] = v
            out.append((k, v))
        return out

    def _emit_waits(self, eng, waits):
        for k, v in waits:
            h = self.semh[k]
            self.prog[eng].append(lambda e, h=h, v=v: e.wait_ge(h, v))
            self.n_wait += 1

    def op(self, eng, fn, reads=(), writes=()):
        self._emit_waits(eng, self._deps(eng, reads, writes))
        self.cnt[eng] += 1
        n = self.cnt[eng]
        h = self.semh[eng]
        self.prog[eng].append(lambda e, fn=fn, h=h: fn(e).then_inc(h, 1))
        self.n_inst += 1
        for b in writes:
            b.w = (eng, n)
            b.r = {}
        for b in reads:
            if b.r.get(eng, 0) < n:
                b.r[eng] = n

    def dma(self, q, fn, reads=(), writes=()):
        waits = self._deps(q, reads, writes)
        keys, rr = self.dq[q]
        k = keys[rr]
        self.dq[q][1] = (rr + 1) % len(keys)
        prev = self.cnt[k]
        if prev > 0 and self.seen[q].get(k, 0) < prev:
            self.seen[q][k] = prev
            waits.append((k, prev))
        self._emit_waits(q, waits)
        self.cnt[k] += 16
        n = self.cnt[k]
        h = self.semh[k]
        self.prog[q].append(lambda e, fn=fn, h=h: fn(e).then_inc(h, 16))
        self.n_inst += 1
        for b in writes:
            b.w = (k, n)
            b.r = {}
        for b in reads:
            if b.r.get(k, 0) < n:
                b.r[k] = n

    def finish(self, bufs):
        need = {}
        for b in bufs:
            if b.w is not None and need.get(b.w[0], 0) < b.w[1]:
                need[b.w[0]] = b.w[1]
            for k, v in b.r.items():
                if need.get(k, 0) < v:
                    need[k] = v
        self._emit_waits("sp", list(need.items()))

    def emit(self):
        nc = self.nc
        with nc.Block() as block:
            @block.tensor
            def _(e):
                for f in self.prog["pe"]:
                    f(e)

            @block.scalar
            def _(e):
                for f in self.prog["act"]:
                    f(e)

            @block.vector
            def _(e):
                for f in self.prog["dve"]:
                    f(e)

            @block.gpsimd
            def _(e):
                for f in self.prog["pool"]:
                    f(e)

            @block.sync
            def _(e):
                for f in self.prog["sp"]:
                    f(e)


def TT(out, in0, in1, op):
    return lambda e: e.tensor_tensor(out=out, in0=in0, in1=in1, op=op)


def TS(out, in0, s1, s2, op0, op1=None):
    if op1 is None:
        return lambda e: e.tensor_scalar(out=out, in0=in0, scalar1=s1, scalar2=None, op0=op0)
    return lambda e: e.tensor_scalar(out=out, in0=in0, scalar1=s1, scalar2=s2, op0=op0, op1=op1)


def STT(out, in0, scalar, in1, op0, op1, accum_out=None):
    if accum_out is None:
        return lambda e: e.scalar_tensor_tensor(out=out, in0=in0, scalar=scalar, in1=in1, op0=op0, op1=op1)
    return lambda e: e.scalar_tensor_tensor(out=out, in0=in0, scalar=scalar, in1=in1, op0=op0, op1=op1,
                                            accum_out=accum_out)


def ACTF(out, in_, func, bias=None, scale=None, accum_out=None):
    kw = {}
    if bias is not None:
        kw["bias"] = bias
    if scale is not None:
        kw["scale"] = scale
    if accum_out is not None:
        kw["accum_out"] = accum_out
    return lambda e: e.activation(out=out, in_=in_, func=func, **kw)


def CP(out, in_):
    return lambda e: e.tensor_copy(out=out, in_=in_)


def MM(out, lhsT, rhs, start, stop):
    return lambda e: e.matmul(out=out, lhsT=lhsT, rhs=rhs, start=start, stop=stop)


def TR(out, in_, ident):
    return lambda e: e.transpose(out=out, in_=in_, identity=ident)


def RED(out, in_, op, axis=AX.X):
    return lambda e: e.tensor_reduce(out=out, in_=in_, axis=axis, op=op)


def DMA(out, in_):
    return lambda e: e.dma_start(out=out, in_=in_)


def GATHER(out, table, idx_col):
    return lambda e: e.indirect_dma_start(
        out=out, out_offset=None, in_=table,
        in_offset=bass.IndirectOffsetOnAxis(ap=idx_col, axis=0))


class T:
    def __init__(self, nc, name, shape, dtype, psum=False):
        if psum:
            self.t = nc.alloc_psum_tensor(name, shape, dtype)
        else:
            self.t = nc.alloc_sbuf_tensor(name, shape, dtype)
        self.b = Buf(name)

    def __getitem__(self, key):
        return self.t[key]


def build(NSEQ, S, debug=False):
    NB = S // 128
    nc = bass.Bass("TRN2", target_bir_lowering=False)

    def din(name, shape):
        return nc.dram_tensor(name, shape, F32, kind="ExternalInput").ap()

    x = din("x", [NSEQ, S, D])
    mix_norm_g = din("mix_norm_g", [1, D])
    w_in = din("w_in", [1, D, 3584])
    pool_w = din("pool_w", [1, 4, 128, 128])
    pool_scale = din("pool_scale", [1, 512])
    w_branch_a = din("w_branch_a", [1, 512, D])
    conv_w = din("conv_w", [1, 31, 512])
    conv_b = din("conv_b", [1, 512])
    conv_ln_g = din("conv_ln_g", [1, 512])
    conv_ln_b = din("conv_ln_b", [1, 512])
    w_branch_b = din("w_branch_b", [1, 512, D])
    gate_b = din("gate_b", [1, 2, D])
    w_out = din("w_out", [1, D, D])
    ffn_norm_g = din("ffn_norm_g", [1, D])
    peer_w_q = din("peer_w_q", [1, D, 2048])
    peer_sub_keys = din("peer_sub_keys", [1, 8, 2, 128, 128])
    peer_u = din("peer_u", [1, 16384, D])
    peer_v = din("peer_v", [1, 16384, D])
    final_norm_g = din("final_norm_g", [D])
    y = nc.dram_tensor("y", [NSEQ, S, D], F32, kind="ExternalOutput").ap()

    S_ = Sync(nc)
    defer = [None]

    def op(eng, fn, reads=(), writes=()):
        if defer[0] is not None:
            defer[0].append((S_.op, eng, fn, tuple(reads), tuple(writes)))
        else:
            S_.op(eng, fn, reads, writes)

    def dma(q, fn, reads=(), writes=()):
        if defer[0] is not None:
            defer[0].append((S_.dma, q, fn, tuple(reads), tuple(writes)))
        else:
            S_.dma(q, fn, reads, writes)

    def sb(name, shape, dtype=F32):
        return T(nc, name, shape, dtype)

    poolw = sb("poolw", [128, 4, 128], BF16)
    skT = sb("skT", [128, 16, 128], BF16)
    NWG = 4
    NWA = 2
    WG = [sb("WG%d" % i, [128, 8, 512], BF16) for i in range(NWG)]
    wg_rr = [0]

    ident_f = sb("ident_f", [128, 128])
    ident_bf = sb("ident_bf", [128, 128], BF16)
    ones_f = sb("ones_f", [128, 128])
    iof_i = sb("iof_i", [128, 128], I32)
    iop_i = sb("iop_i", [128, 1], I32)
    iof = sb("iof", [128, 128])
    iop = sb("iop", [128, 1])
    rcfix = sb("rcfix", [128, 4, 15])
    rc_i = sb("rc_i", [128, 4, 15], I32)
    T1 = sb("T1", [128, 128])
    T2 = sb("T2", [128, 128])
    cwT = sb("cwT", [128, 124])
    vT = sb("vT", [128, 40])
    g2bc = sb("g2bc", [128, D])
    gfbc = sb("gfbc", [128, D])
    sknat = [sb("sknat%d" % i, [128, 128], BF16) for i in range(2)]
    eps_t = sb("eps_t", [128, 1])

    PT = T(nc, "PT", [128, 1024], BF16, psum=True)
    PF = nc.alloc_psum_tensor("PF", [128, 7, 512], F32)
    bPF = [Buf("PF%d" % i) for i in range(7)]

    def pf2(i):
        return PF[:, i:i + 2, :].rearrange("p b f -> p (b f)")

    op("pool", lambda e: e.iota(out=iof_i[:], pattern=[[1, 128]], base=0, channel_multiplier=0), writes=[iof_i.b])
    op("pool", lambda e: e.iota(out=iop_i[:], pattern=[[0, 1]], base=0, channel_multiplier=1), writes=[iop_i.b])
    op("pool", lambda e: e.iota(out=rc_i[:], pattern=[[0, 4], [1, 15]], base=1, channel_multiplier=0), writes=[rc_i.b])
    op("dve", CP(iof[:], iof_i[:]), reads=[iof_i.b], writes=[iof.b])
    op("dve", CP(iop[:], iop_i[:]), reads=[iop_i.b], writes=[iop.b])
    op("dve", TS(ident_f[:], iof[:], iop[:, 0:1], None, ALU.is_equal), reads=[iof.b, iop.b], writes=[ident_f.b])
    op("dve", CP(ident_bf[:], ident_f[:]), reads=[ident_f.b], writes=[ident_bf.b])
    op("dve", lambda e: e.memset(ones_f[:], 1.0 / 512.0), writes=[ones_f.b])
    op("dve", lambda e: e.memset(eps_t[:], EPS), writes=[eps_t.b])
    op("dve", CP(rcfix[:], rc_i[:]), reads=[rc_i.b], writes=[rcfix.b])
    for g in range(4):
        w = float(2 ** (g + 1))
        op("dve", TS(rcfix[:, g, :], rcfix[:, g, :], w, None, ALU.min), reads=[rcfix.b], writes=[rcfix.b])
    op("dve", lambda e: e.reciprocal(out=rcfix[:], in_=rcfix[:]), reads=[rcfix.b], writes=[rcfix.b])

    op("dve", lambda e: e.memset(T1[:], 0.0), writes=[T1.b])
    op("dve", lambda e: e.memset(T2[:], 0.0), writes=[T2.b])
    dma("sp", DMA(T1[0:124, :], conv_w[0].rearrange("k (c p) -> (k c) p", p=128)), writes=[T1.b])
    row = 0
    for src, n in ((mix_norm_g[0], 8), (pool_scale[0], 4), (conv_b[0], 4), (conv_ln_g[0], 4), (conv_ln_b[0], 4),
                   (gate_b[0, 0], 8), (gate_b[0, 1], 8)):
        dma("sp", DMA(T2[row:row + n, :], src.rearrange("(c p) -> c p", p=128)), writes=[T2.b])
        row += n
    op("pe", TR(PF[:, 0, 0:128], T1[:], ident_f[:]), reads=[T1.b, ident_f.b], writes=[bPF[0]])
    op("pe", TR(PF[:, 0, 128:256], T2[:], ident_f[:]), reads=[T2.b, ident_f.b], writes=[bPF[0]])
    op("dve", CP(cwT[:], PF[:, 0, 0:124]), reads=[bPF[0]], writes=[cwT.b])
    op("dve", CP(vT[:], PF[:, 0, 128:168]), reads=[bPF[0]], writes=[vT.b])
    g1T, pscT, cbT, lgT, lbT, gbT = vT[:, 0:8], vT[:, 8:12], vT[:, 12:16], vT[:, 16:20], vT[:, 20:24], vT[:, 24:40]

    dma("sp", DMA(g2bc[:], ffn_norm_g[0].partition_broadcast(128)), writes=[g2bc.b])
    dma("sp", DMA(gfbc[:], final_norm_g.partition_broadcast(128)), writes=[gfbc.b])

    for c in range(4):
        dma("pool", DMA(poolw[:, c, :], pool_w[0, c]), writes=[poolw.b])
    for m in range(16):
        sn = sknat[m % 2]
        dma("pool", DMA(sn[:], peer_sub_keys[0, m // 2, m % 2]), writes=[sn.b])
        op("pe", TR(PT[:, 0:128], sn[:], ident_bf[:]), reads=[sn.b, ident_bf.b], writes=[PT.b])
        op("dve", CP(skT[:, m, :], PT[:, 0:128]), reads=[PT.b], writes=[skT.b])

    NG = 10
    gb = [sb("gb%d" % i, [128, 2 * D], BF16) for i in range(NG)]
    uv = nc.dram_tensor("uv_scr", [16384, 2 * D], BF16, kind="Internal").ap()
    win_bf = nc.dram_tensor("win_scr", [D, 3584], BF16, kind="Internal").ap()
    wq_bf = nc.dram_tensor("wq_scr", [D, 2048], BF16, kind="Internal").ap()
    b_uv = [Buf("uv%d" % j) for j in range(128)]
    b_win = [Buf("win%d" % j) for j in range(7)]
    b_wq = [Buf("wq%d" % j) for j in range(4)]
    wout_bf = nc.dram_tensor("wout_scr", [D, D], BF16, kind="Internal").ap()
    wab_bf = nc.dram_tensor("wab_scr", [2, 512, D], BF16, kind="Internal").ap()
    b_wout = [Buf("wout%d" % j) for j in range(2)]
    b_wab = [Buf("wab%d" % j) for j in range(2)]

    def wg4(w):
        return w[:].rearrange("p c f -> p (c f)").rearrange("p (g d) -> p g d", g=4)

    for j in range(7):
        dma("pool", DMA(win_bf[:, j * 512:(j + 1) * 512], w_in[0][:, j * 512:(j + 1) * 512]), writes=[b_win[j]])
    for j, src in enumerate((w_branch_a, w_branch_b)):
        dma("pool", DMA(wab_bf[j], src[0]), writes=[b_wab[j]])
    for j in range(2):
        dma("pool", DMA(wout_bf[:, j * 512:(j + 1) * 512], w_out[0][:, j * 512:(j + 1) * 512]), writes=[b_wout[j]])
    for j in range(4):
        dma("pool", DMA(wq_bf[:, j * 512:(j + 1) * 512], peer_w_q[0][:, j * 512:(j + 1) * 512]), writes=[b_wq[j]])
    for j in range(16):
        r0, r1 = j * 1024, (j + 1) * 1024
        dma("pool", DMA(uv[r0:r1, 0:D], peer_u[0, r0:r1, :]), writes=[b_uv[2 * j]])
        dma("pool", DMA(uv[r0:r1, D:2 * D], peer_v[0, r0:r1, :]), writes=[b_uv[2 * j + 1]])

    xt = [sb("xt%d" % i, [128, D]) for i in range(3)]
    junk_a = sb("junk_a", [128, D], BF16)
    junk_d = sb("junk_d", [128, D], BF16)
    stA = sb("stA", [128, 1])
    stC = sb("stC", [128, 1])
    stF = sb("stF", [128, 1])
    xn = sb("xn", [128, D], BF16)
    hT = sb("hT", [128, 8, 128], BF16)
    zap = sb("zap", [128, 4, 143])
    sA = sb("sA", [128, 4, 143])
    sB = sb("sB", [128, 4, 143])
    pooled = sb("pooled", [128, 4, 128], BF16)
    ptmp = sb("ptmp", [128, 4, 15])
    paT = sb("paT", [128, 4, 128], BF16)
    tA = sb("tA", [128, 4, 128])
    ubp = sb("ubp", [128, 4, 158], BF16)
    dgw = sb("dgw", [128, 124, 128], BF16)
    acc = sb("acc", [128, 4, 128])
    lnm = sb("lnm", [128, 4, 128])
    ub2 = sb("ub2", [128, 4, 128], BF16)
    gtA = sb("gtA", [128, 8, 128])
    gtB = sb("gtB", [128, 8, 128])
    merged = sb("merged", [128, 8, 128], BF16)
    hb2 = [sb("hb%d" % i, [128, D], BF16) for i in range(2)]
    hbT = sb("hbT", [128, 8, 128], BF16)
    qT = sb("qT", [128, 16, 128], BF16)
    s2 = [sb("s2_%d" % i, [128, 256]) for i in range(2)]
    vals = sb("vals", [128, 16, 16])
    idxu = sb("idxu", [128, 16, 16], U32)
    idxf = sb("idxf", [128, 16, 16])
    cand = sb("cand", [128, 8, 256])
    best = sb("best", [128, 8, 16])
    posu = sb("posu", [128, 8, 16], U32)
    ku = sb("ku", [128, 2, 128], U32)
    kf = sb("kf", [128, 2, 128])
    oh = sb("oh", [128, 8, 256], BF16)
    isel = sb("isel", [128, 2, 128])
    eidf = sb("eidf", [128, 128])
    eidi2 = [sb("eidi%d" % i, [128, 128], I32) for i in range(2)]
    gte = sb("gte", [128, 8, 16])
    gsum = sb("gsum", [128, 8])
    gate2 = [sb("gate%d" % i, [128, 128]) for i in range(2)]
    actv = sb("actv", [128, 128])
    gl = sb("gl", [128, 128])
    coef = sb("coef", [128, 128])
    GS = 2
    NGRP = 128 // GS
    b_act = [Buf("act%d" % i) for i in range(128)]
    b_gl = [Buf("gl%d" % i) for i in range(NGRP)]
    b_coef = [Buf("coef%d" % i) for i in range(NGRP)]
    dg = [sb("dg%d" % i, [128, 128], BF16) for i in range(6)]
    print("SBUF bytes remaining/partition:", nc.sbuf_bytes_remaining)

    op("dve", lambda e: e.memset(zap[:], 0.0), writes=[zap.b])
    op("dve", lambda e: e.memset(ubp[:], 0.0), writes=[ubp.b])
    op("dve", TT(dgw[:], ident_f[:].unsqueeze(1).to_broadcast([128, 124, 128]),
                 cwT[:].unsqueeze(2).to_broadcast([128, 124, 128]), ALU.mult), reads=[ident_f.b, cwT.b], writes=[dgw.b])

    blocks = [(s, i) for s in range(NSEQ) for i in range(NB)]

    def load_x(bi):
        if bi >= len(blocks):
            return
        s, i = blocks[bi]
        t = xt[bi % 3]
        dma("sp", DMA(t[:], x[s, i * 128:(i + 1) * 128, :]), writes=[t.b])

    def rstd_from_ss(st):
        c = st[:, 0:1]
        op("act", ACTF(c, c, AF.Ln, bias=eps_t[:, 0:1], scale=1.0 / D), reads=[st.b, eps_t.b], writes=[st.b])
        op("act", ACTF(c, c, AF.Exp, scale=-0.5), reads=[st.b], writes=[st.b])

    ringA = [0]
    ringC = [0]

    def ring_next(chain):
        if chain == "A":
            w = WG[ringA[0] % NWA]
            ringA[0] += 1
        else:
            w = WG[NWA + ringC[0] % (NWG - NWA)]
            ringC[0] += 1
        return w

    def load_wg(src, sbuf, col0, chain="A"):
        w = ring_next(chain)
        dma("sp", DMA(w[:], src[:, col0:col0 + 512].rearrange("(c p) f -> p c f", p=128)), reads=[sbuf[col0 // 512]],
            writes=[w.b])
        return w

    def load_wab(j):
        w = ring_next("A")
        dma("sp", DMA(wg4(w), wab_bf[j].rearrange("(g p) d -> p g d", p=128)), reads=[b_wab[j]], writes=[w.b])
        return w

    def proj4(w, rhsT, bank, rhs_b):
        for j in range(4):
            for c in range(8):
                op("pe", MM(PF[:, bank, j * 128:(j + 1) * 128], w[:, c, j * 128:(j + 1) * 128], rhsT[:, c, :],
                            c == 0, c == 7), reads=[w.b, rhs_b], writes=[bPF[bank]])

    PTA = PF[:, 1, :].bitcast(BF16)
    PCF = PT[:].bitcast(F32)

    def proAB(bi):
        s, i = blocks[bi]
        first = (i == 0)
        X = xt[bi % 3]

        op("act", ACTF(junk_a[:], X[:], AF.Square, accum_out=stA[:, 0:1]), reads=[X.b], writes=[stA.b, junk_a.b])
        rstd_from_ss(stA)
        op("act", ACTF(xn[:], X[:], AF.Copy, scale=stA[:, 0:1]), reads=[X.b, stA.b], writes=[xn.b])
        yield
        for c in range(8):
            op("pe", TR(PTA[:, c * 128:(c + 1) * 128], xn[:, c * 128:(c + 1) * 128], ident_bf[:]),
               reads=[xn.b, ident_bf.b], writes=[bPF[1]])
        op("dve", TT(hT[:], PTA.rearrange("p (c t) -> p c t", c=8), g1T.unsqueeze(2).to_broadcast([128, 8, 128]),
                     ALU.mult), reads=[bPF[1], vT.b], writes=[hT.b])
        yield

        w = load_wg(win_bf, b_win, 0)
        proj4(w, hT, 0, hT.b)
        yield
        op("act", ACTF(zap[:, :, 15:143], PF[:, 0, :].rearrange("p (g t) -> p g t", g=4), AF.Copy),
           reads=[bPF[0]], writes=[zap.b])
        op("dve", TT(sA[:, :, 1:143], zap[:, :, 1:143], zap[:, :, 0:142], ALU.add), reads=[zap.b], writes=[sA.b])
        op("dve", TT(sB[:, 1:4, 3:143], sA[:, 1:4, 3:143], sA[:, 1:4, 1:141], ALU.add), reads=[sA.b], writes=[sB.b])
        yield
        op("dve", TT(sA[:, 2:4, 7:143], sB[:, 2:4, 7:143], sB[:, 2:4, 3:139], ALU.add), reads=[sB.b], writes=[sA.b])
        op("dve", TT(sB[:, 3:4, 15:143], sA[:, 3:4, 15:143], sA[:, 3:4, 7:135], ALU.add), reads=[sA.b], writes=[sB.b])
        yield
        srcs = (sA, sB, sA, sB)
        for g in range(4):
            wdw = float(2 ** (g + 1))
            sg = srcs[g]
            op("dve", STT(pooled[:, g, :], sg[:, g, 15:143], 1.0 / wdw, zap[:, g, 15:143], ALU.mult, ALU.subtract),
               reads=[sg.b, zap.b], writes=[pooled.b])
        yield
        if first:
            for g in range(4):
                sg = srcs[g]
                op("dve", TT(ptmp[:, g, :], sg[:, g, 15:30], rcfix[:, g, :], ALU.mult), reads=[sg.b, rcfix.b],
                   writes=[ptmp.b])
            op("dve", TT(pooled[:, :, 0:15], ptmp[:], zap[:, :, 15:30], ALU.subtract), reads=[ptmp.b, zap.b],
               writes=[pooled.b])
            yield
        if i + 1 < NB:
            op("act", ACTF(zap[:, :, 0:15], zap[:, :, 128:143], AF.Copy), reads=[zap.b], writes=[zap.b])
        else:
            op("dve", lambda e: e.memset(zap[:, :, 0:15], 0.0), writes=[zap.b])
        for g in range(4):
            op("pe", MM(PF[:, 0, g * 128:(g + 1) * 128], poolw[:, g, :], pooled[:, g, :], True, True),
               reads=[poolw.b, pooled.b], writes=[bPF[0]])
        op("dve", TT(paT[:], PF[:, 0, :].rearrange("p (g t) -> p g t", g=4),
                     pscT.unsqueeze(2).to_broadcast([128, 4, 128]), ALU.mult), reads=[bPF[0], vT.b], writes=[paT.b])
        yield
        YA = pf2(3)
        wA = load_wab(0)
        Wa = wg4(wA)
        for dc in range(8):
            for g in range(4):
                op("pe", MM(YA[:, dc * 128:(dc + 1) * 128], Wa[:, g, dc * 128:(dc + 1) * 128], paT[:, g, :],
                            g == 0, g == 3), reads=[wA.b, paT.b], writes=[bPF[3], bPF[4]])
            if dc % 4 == 3:
                yield
        for half in range(2):
            w = load_wg(win_bf, b_win, 1536 + half * 512)
            proj4(w, hT, half, hT.b)
            yield
            for j in range(4):
                dc = half * 4 + j
                op("act", ACTF(gtA[:, dc, :], PF[:, half, j * 128:(j + 1) * 128], AF.Sigmoid,
                               bias=gbT[:, dc:dc + 1]), reads=[bPF[half], vT.b], writes=[gtA.b])
            yield
        op("dve", TT(gtA[:].rearrange("p c t -> p (c t)"), gtA[:].rearrange("p c t -> p (c t)"), YA, ALU.mult),
           reads=[gtA.b, bPF[3], bPF[4]], writes=[gtA.b])
        yield

        w = load_wg(win_bf, b_win, 512)
        proj4(w, hT, 1, hT.b)
        yield
        w = load_wg(win_bf, b_win, 1024)
        proj4(w, hT, 2, hT.b)
        yield
        op("act", ACTF(tA[:], PF[:, 2, :].rearrange("p (g t) -> p g t", g=4), AF.Sigmoid), reads=[bPF[2]],
           writes=[tA.b])
        op("dve", TT(ubp[:, :, 30:158], PF[:, 1, :].rearrange("p (g t) -> p g t", g=4), tA[:], ALU.mult),
           reads=[bPF[1], tA.b], writes=[ubp.b])
        yield
        for c in range(4):
            for k in range(31):
                op("pe", MM(PF[:, 2, c * 128:(c + 1) * 128], dgw[:, k * 4 + c, :], ubp[:, c, k:k + 128], k == 0, k == 30),
                   reads=[dgw.b, ubp.b], writes=[bPF[2]])
            yield
        op("dve", TT(acc[:], PF[:, 2, :].rearrange("p (g t) -> p g t", g=4),
                     cbT.unsqueeze(2).to_broadcast([128, 4, 128]), ALU.add), reads=[bPF[2], vT.b], writes=[acc.b])
        if i + 1 < NB:
            op("act", ACTF(ubp[:, :, 0:30], ubp[:, :, 128:158], AF.Copy), reads=[ubp.b], writes=[ubp.b])
        else:
            op("dve", lambda e: e.memset(ubp[:, :, 0:30], 0.0), writes=[ubp.b])
        op("act", ACTF(tA[:], acc[:], AF.Square), reads=[acc.b], writes=[tA.b])
        for c in range(4):
            op("pe", MM(PF[:, 1, 0:128], ones_f[:], acc[:, c, :], c == 0, c == 3), reads=[ones_f.b, acc.b],
               writes=[bPF[1]])
        for c in range(4):
            op("pe", MM(PF[:, 1, 128:256], ones_f[:], tA[:, c, :], c == 0, c == 3), reads=[ones_f.b, tA.b],
               writes=[bPF[1]])
        yield
        op("dve", CP(lnm[:, 0, :], PF[:, 1, 0:128]), reads=[bPF[1]], writes=[lnm.b])
        op("dve", TT(lnm[:, 1, :], lnm[:, 0, :], lnm[:, 0, :], ALU.mult), reads=[lnm.b], writes=[lnm.b])
        op("dve", TT(lnm[:, 2, :], PF[:, 1, 128:256], lnm[:, 1, :], ALU.subtract), reads=[bPF[1], lnm.b],
           writes=[lnm.b])
        op("act", ACTF(lnm[:, 2, :], lnm[:, 2, :], AF.Sqrt, bias=eps_t[:, 0:1]), reads=[lnm.b, eps_t.b],
           writes=[lnm.b])
        op("dve", lambda e: e.reciprocal(out=lnm[:, 3, :], in_=lnm[:, 2, :]), reads=[lnm.b], writes=[lnm.b])
        yield
        op("dve", TT(acc[:], acc[:], lnm[:, 0:1, :].to_broadcast([128, 4, 128]), ALU.subtract),
           reads=[acc.b, lnm.b], writes=[acc.b])
        op("dve", TT(acc[:], acc[:], lnm[:, 3:4, :].to_broadcast([128, 4, 128]), ALU.mult),
           reads=[acc.b, lnm.b], writes=[acc.b])
        yield
        op("dve", TT(acc[:], acc[:], lgT.unsqueeze(2).to_broadcast([128, 4, 128]), ALU.mult),
           reads=[acc.b, vT.b], writes=[acc.b])
        op("dve", TT(acc[:], acc[:], lbT.unsqueeze(2).to_broadcast([128, 4, 128]), ALU.add),
           reads=[acc.b, vT.b], writes=[acc.b])
        op("act", ACTF(tA[:], acc[:], AF.Sigmoid), reads=[acc.b], writes=[tA.b])
        op("dve", TT(ub2[:], acc[:], tA[:], ALU.mult), reads=[acc.b, tA.b], writes=[ub2.b])
        yield
        YB = pf2(3)
        wB = load_wab(1)
        Wb = wg4(wB)
        for dc in range(8):
            for c in range(4):
                op("pe", MM(YB[:, dc * 128:(dc + 1) * 128], Wb[:, c, dc * 128:(dc + 1) * 128], ub2[:, c, :],
                            c == 0, c == 3), reads=[wB.b, ub2.b], writes=[bPF[3], bPF[4]])
            if dc % 4 == 3:
                yield
        for half in range(2):
            w = load_wg(win_bf, b_win, 2560 + half * 512)
            proj4(w, hT, half, hT.b)
            yield
            for j in range(4):
                dc = half * 4 + j
                op("act", ACTF(gtB[:, dc, :], PF[:, half, j * 128:(j + 1) * 128], AF.Sigmoid,
                               bias=gbT[:, 8 + dc:8 + dc + 1]), reads=[bPF[half], vT.b], writes=[gtB.b])
            yield
        op("dve", TT(gtB[:].rearrange("p c t -> p (c t)"), gtB[:].rearrange("p c t -> p (c t)"), YB, ALU.mult),
           reads=[gtB.b, bPF[3], bPF[4]], writes=[gtB.b])
        op("dve", TT(merged[:], gtA[:], gtB[:], ALU.add), reads=[gtA.b, gtB.b], writes=[merged.b])
        yield
        PO = pf2(3)
        for half in range(2):
            wO = load_wg(wout_bf, b_wout, half * 512)
            for c in range(8):
                op("pe", MM(PO[:, half * 512:(half + 1) * 512], merged[:, c, :], wO[:, c, :],
                            c == 0, c == 7), reads=[merged.b, wO.b], writes=[bPF[3 + half]])
            yield
        op("dve", TT(X[:], X[:], PO, ALU.add), reads=[X.b, bPF[3], bPF[4]], writes=[X.b])
        yield

    def proC(bi):
        X = xt[bi % 3]
        hb = hb2[bi % 2]
        eidi = eidi2[bi % 2]
        gate = gate2[bi % 2]
        op("act", ACTF(junk_a[:], X[:], AF.Square, accum_out=stC[:, 0:1]), reads=[X.b], writes=[stC.b, junk_a.b])
        rstd_from_ss(stC)
        op("dve", STT(hb[:], X[:], stC[:, 0:1], g2bc[:], ALU.mult, ALU.mult), reads=[X.b, stC.b, g2bc.b],
           writes=[hb.b])
        yield
        for c in range(8):
            op("pe", TR(PT[:, c * 128:(c + 1) * 128], hb[:, c * 128:(c + 1) * 128], ident_bf[:]),
               reads=[hb.b, ident_bf.b], writes=[PT.b])
        op("act", ACTF(hbT[:], PT[:].rearrange("p (c t) -> p c t", c=8), AF.Copy), reads=[PT.b], writes=[hbT.b])
        yield
        for mg in range(4):
            w = load_wg(wq_bf, b_wq, mg * 512, chain="C")
            for j in range(4):
                for c in range(8):
                    op("pe", MM(PCF[:, j * 128:(j + 1) * 128], w[:, c, j * 128:(j + 1) * 128], hbT[:, c, :],
                                c == 0, c == 7), reads=[w.b, hbT.b], writes=[PT.b])
            op("act", ACTF(qT[:, mg * 4:(mg + 1) * 4, :], PCF.rearrange("p (j t) -> p j t", j=4), AF.Copy),
               reads=[PT.b], writes=[qT.b])
            yield
        for mg in range(4):
            for j in range(4):
                m = mg * 4 + j
                op("pe", MM(PCF[:, j * 128:(j + 1) * 128], qT[:, m, :], skT[:, m, :], True, True),
                   reads=[qT.b, skT.b], writes=[PT.b])
            for j in range(4):
                m = mg * 4 + j
                sv = PCF[:, j * 128:(j + 1) * 128]
                bb = PT.b
                s2t = s2[m % 2]
                op("dve", lambda e, sv=sv, m=m: e.max(out=vals[:, m, 0:8], in_=sv), reads=[bb], writes=[vals.b])
                op("dve", lambda e, sv=sv, m=m: e.max_index(out=idxu[:, m, 0:8], in_max=vals[:, m, 0:8], in_values=sv),
                   reads=[bb, vals.b], writes=[idxu.b])
                op("dve", lambda e, sv=sv, m=m, s2t=s2t: e.match_replace(out=s2t[:, 0:128],
                                                                       in_to_replace=vals[:, m, 0:8],
                                                                       in_values=sv, imm_value=NEG),
                   reads=[bb, vals.b], writes=[s2t.b])
                op("dve", lambda e, m=m, s2t=s2t: e.max(out=vals[:, m, 8:16], in_=s2t[:, 0:128]), reads=[s2t.b],
                   writes=[vals.b])
                op("dve", lambda e, m=m, s2t=s2t: e.max_index(out=idxu[:, m, 8:16], in_max=vals[:, m, 8:16],
                                                              in_values=s2t[:, 0:128]),
                   reads=[s2t.b, vals.b], writes=[idxu.b])
            yield
        op("dve", CP(idxf[:], idxu[:]), reads=[idxu.b], writes=[idxf.b])
        v4 = vals[:].rearrange("p (h two) k -> p h two k", two=2)
        i4 = idxf[:].rearrange("p (h two) k -> p h two k", two=2)
        c4 = cand[:].rearrange("p h (a b) -> p h a b", a=16)
        op("dve", TT(c4, v4[:, :, 0, :].unsqueeze(3).to_broadcast([128, 8, 16, 16]),
                     v4[:, :, 1, :].unsqueeze(2).to_broadcast([128, 8, 16, 16]), ALU.add),
           reads=[vals.b], writes=[cand.b])
        yield
        for h in range(8):
            s2t = s2[h % 2]
            ch = cand[:, h, :]
            op("dve", lambda e, ch=ch, h=h: e.max(out=best[:, h, 0:8], in_=ch), reads=[cand.b], writes=[best.b])
            op("dve", lambda e, ch=ch, h=h: e.max_index(out=posu[:, h, 0:8], in_max=best[:, h, 0:8], in_values=ch),
               reads=[cand.b, best.b], writes=[posu.b])
            op("dve", lambda e, ch=ch, h=h, s2t=s2t: e.match_replace(out=s2t[:], in_to_replace=best[:, h, 0:8],
                                                                   in_values=ch, imm_value=NEG),
               reads=[cand.b, best.b], writes=[s2t.b])
            op("dve", lambda e, h=h, s2t=s2t: e.max(out=best[:, h, 8:16], in_=s2t[:]), reads=[s2t.b], writes=[best.b])
            op("dve", lambda e, h=h, s2t=s2t: e.max_index(out=posu[:, h, 8:16], in_max=best[:, h, 8:16],
                                                          in_values=s2t[:]),
               reads=[s2t.b, best.b], writes=[posu.b])
            yield
        pu = posu[:].rearrange("p h k -> p (h k)")
        op("dve", lambda e: e.tensor_single_scalar(out=ku[:, 0, :], in_=pu, scalar=4, op=ALU.logical_shift_right),
           reads=[posu.b], writes=[ku.b])
        op("dve", lambda e: e.tensor_single_scalar(out=ku[:, 1, :], in_=pu, scalar=15, op=ALU.bitwise_and),
           reads=[posu.b], writes=[ku.b])
        op("dve", CP(kf[:], ku[:]), reads=[ku.b], writes=[kf.b])
        yield
        o4 = oh[:].rearrange("p h (a b) -> p h a b", a=16)
        for half in range(2):
            k3 = kf[:, half, :].rearrange("p (h k) -> p h k", h=8)
            op("dve", TT(o4, k3.unsqueeze(3).to_broadcast([128, 8, 16, 16]),
                         iof[:, 0:16].unsqueeze(1).unsqueeze(1).to_broadcast([128, 8, 16, 16]), ALU.is_equal),
               reads=[kf.b, iof.b], writes=[oh.b])
            yield
            op("dve", TT(o4, o4, i4[:, :, half, :].unsqueeze(2).to_broadcast([128, 8, 16, 16]), ALU.mult),
               reads=[oh.b, idxf.b], writes=[oh.b])
            yield
            op("dve", RED(isel[:, half, :], oh[:].rearrange("p h (a b) -> p (h a) b", a=16), ALU.add),
               reads=[oh.b], writes=[isel.b])
            yield
        op("dve", STT(eidf[:], isel[:, 0, :], 128.0, isel[:, 1, :], ALU.mult, ALU.add), reads=[isel.b], writes=[eidf.b])
        op("dve", CP(eidi[:], eidf[:]), reads=[eidf.b], writes=[eidi.b])
        op("dve", TT(gte[:], best[:], best[:, :, 0:1].to_broadcast([128, 8, 16]), ALU.subtract), reads=[best.b],
           writes=[gte.b])
        op("act", ACTF(gte[:], gte[:], AF.Exp), reads=[gte.b], writes=[gte.b])
        yield
        op("dve", RED(gsum[:], gte[:], ALU.add), reads=[gte.b], writes=[gsum.b])
        op("dve", lambda e: e.reciprocal(out=gsum[:], in_=gsum[:]), reads=[gsum.b], writes=[gsum.b])
        op("dve", TT(gate[:].rearrange("p (h k) -> p h k", h=8), gte[:], gsum[:].unsqueeze(2).to_broadcast([128, 8, 16]),
                     ALU.mult), reads=[gte.b, gsum.b], writes=[gate.b])
        yield

    gbrr = [0]

    def main(bi, thunks):
        s, i = blocks[bi]
        X = xt[bi % 3]
        hb = hb2[bi % 2]
        eidi = eidi2[bi % 2]
        gate = gate2[bi % 2]
        PA = pf2(5)
        pos = [0]
        per = -(-len(thunks) // 112) if thunks else 0

        def advance(n):
            for _ in range(n):
                if pos[0] >= len(thunks):
                    return
                f, a, fn, r, w = thunks[pos[0]]
                pos[0] += 1
                f(a, fn, r, w)

        for grp in range(NGRP):
            sl0 = grp * GS
            bufs = []
            for sl in range(sl0, sl0 + GS):
                g = gb[gbrr[0] % NG]
                gbrr[0] += 1
                bufs.append(g)
                dma("pool", GATHER(g[:], uv, eidi[:, sl:sl + 1]), reads=[eidi.b] + (b_uv if bi == 0 and sl == 0 else []),
                    writes=[g.b])
                op("dve", STT(junk_d[:], g[:, 0:D], 1.0, hb[:], ALU.mult, ALU.mult, accum_out=actv[:, sl:sl + 1]),
                   reads=[g.b, hb.b], writes=[b_act[sl], junk_d.b])
                advance(per)
            op("act", ACTF(gl[:, sl0:sl0 + GS], actv[:, sl0:sl0 + GS], AF.Gelu_apprx_tanh), reads=b_act[sl0:sl0 + GS],
               writes=[b_gl[grp]])
            for k, sl in enumerate(range(sl0, sl0 + GS)):
                g = bufs[k]
                dgt = dg[sl % 6]
                op("act", ACTF(coef[:, sl:sl + 1], gl[:, sl:sl + 1], AF.Copy, scale=gate[:, sl:sl + 1]),
                   reads=[b_gl[grp], gate.b], writes=[b_coef[grp]])
                op("act", ACTF(dgt[:], ident_bf[:], AF.Copy, scale=coef[:, sl:sl + 1]), reads=[ident_bf.b, b_coef[grp]],
                   writes=[dgt.b])
                for half in range(2):
                    op("pe", MM(PA[:, half * 512:(half + 1) * 512], dgt[:], g[:, D + half * 512:D + (half + 1) * 512],
                                sl == 0, sl == 127), reads=[dgt.b, g.b], writes=[bPF[5 + half]])
        advance(len(thunks))
        op("dve", TT(X[:], X[:], PA, ALU.add), reads=[X.b, bPF[5], bPF[6]], writes=[X.b])
        op("act", ACTF(junk_a[:], X[:], AF.Square, accum_out=stF[:, 0:1]), reads=[X.b], writes=[stF.b, junk_a.b])
        rstd_from_ss(stF)
        op("dve", STT(X[:], X[:], stF[:, 0:1], gfbc[:], ALU.mult, ALU.mult), reads=[X.b, stF.b, gfbc.b], writes=[X.b])
        dma("sp", DMA(y[s, i * 128:(i + 1) * 128, :], X[:]), reads=[X.b])
        load_x(bi + 3)

    def collect(gen):
        lst = []
        defer[0] = lst
        for _ in gen:
            pass
        defer[0] = None
        return lst

    def merge(a, b):
        out = []
        ia = ib = 0
        while ia < len(a) or ib < len(b):
            if ib >= len(b) or (ia < len(a) and ia * len(b) <= ib * len(a)):
                out.append(a[ia])
                ia += 1
            else:
                out.append(b[ib])
                ib += 1
        return out

    nblk = len(blocks)
    load_x(0)
    load_x(1)
    load_x(2)
    for _ in proAB(0):
        pass
    for _ in proC(0):
        pass
    if nblk > 1:
        for _ in proAB(1):
            pass
    for bi in range(nblk):
        tc = collect(proC(bi + 1)) if bi + 1 < nblk else []
        tab = collect(proAB(bi + 2)) if bi + 2 < nblk else []
        main(bi, merge(tc, tab))

    S_.finish([t.b for t in xt])
    S_.emit()
    return nc, S_


_CACHE = {}


def kernel(**inputs):
    x = np.ascontiguousarray(np.asarray(inputs["x"], dtype=np.float32))
    B, S, _ = x.shape
    nseq = B // NCORES
    key = (nseq, S)
    if key not in _CACHE:
        _CACHE[key] = build(nseq, S)[0]
    nc = _CACHE[key]
    shared = {k: np.ascontiguousarray(np.asarray(v, dtype=np.float32)) for k, v in inputs.items() if k != "x"}
    in_maps = []
    for c in range(NCORES):
        m = dict(shared)
        m["x"] = x[c * nseq:(c + 1) * nseq]
        in_maps.append(m)
    res = run_bass_kernel_spmd(nc, in_maps, core_ids=list(range(NCORES)))
    return np.concatenate([r["y"] for r in res.results], axis=0)
```

```python
import numpy as np
import concourse.bass as bass
import concourse.mybir as mybir
from concourse.bass_utils import run_bass_kernel_spmd

F32 = mybir.dt.float32
BF16 = mybir.dt.bfloat16
I32 = mybir.dt.int32
U32 = mybir.dt.uint32
ALU = mybir.AluOpType
AF = mybir.ActivationFunctionType
AX = mybir.AxisListType

D = 1024
NCORES = 8
EPS = 1e-6
NEG = -1.0e30


class Buf:
    __slots__ = ("name", "w", "r")

    def __init__(self, name):
        self.name = name
        self.w = None
        self.r = {}


class Sync:
    ENG = ("pe", "act", "dve", "pool", "sp")

    def __init__(self, nc, n_dma_sp=8, n_dma_pool=24, n_dma_act=2):
        self.nc = nc
        self.semh = {}
        self.cnt = {}
        self.prog = {e: [] for e in self.ENG}
        self.seen = {e: {} for e in self.ENG}
        for e in self.ENG:
            self.semh[e] = nc.alloc_semaphore(name="s_" + e)
            self.cnt[e] = 0
        self.dq = {}
        for q, n in (("sp", n_dma_sp), ("pool", n_dma_pool), ("act", n_dma_act)):
            keys = []
            for i in range(n):
                k = ("dma", q, i)
                self.semh[k] = nc.alloc_semaphore(name="d_%s_%d" % (q, i))
                self.cnt[k] = 0
                keys.append(k)
            self.dq[q] = [keys, 0]
        self.n_inst = 0
        self.n_wait = 0

    def _deps(self, eng, reads, writes):
        need = {}
        for b in reads:
            if b.w is not None and need.get(b.w[0], 0) < b.w[1]:
                need[b.w[0]] = b.w[1]
        for b in writes:
            if b.w is not None and need.get(b.w[0], 0) < b.w[1]:
                need[b.w[0]] = b.w[1]
            for k, v in b.r.items():
                if need.get(k, 0) < v:
                    need[k] = v
        seen = self.seen[eng]
        out = []
        for k, v in need.items():
            if eng == "pe" and k == "pe":
                continue
            if seen.get(k, 0) >= v:
                continue
            seen[k] = v
            out.append((k, v))
        return out

    def _emit_waits(self, eng, waits):
        for k, v in waits:
            h = self.semh[k]
            self.prog[eng].append(lambda e, h=h, v=v: e.wait_ge(h, v))
            self.n_wait += 1

    def op(self, eng, fn, reads=(), writes=()):
        self._emit_waits(eng, self._deps(eng, reads, writes))
        self.cnt[eng] += 1
        n = self.cnt[eng]
        h = self.semh[eng]
        self.prog[eng].append(lambda e, fn=fn, h=h: fn(e).then_inc(h, 1))
        self.n_inst += 1
        for b in writes:
            b.w = (eng, n)
            b.r = {}
        for b in reads:
            if b.r.get(eng, 0) < n:
                b.r[eng] = n

    def dma(self, q, fn, reads=(), writes=()):
        waits = self._deps(q, reads, writes)
        keys, rr = self.dq[q]
        k = keys[rr]
        self.dq[q][1] = (rr + 1) % len(keys)
        prev = self.cnt[k]
        if prev > 0 and self.seen[q].get(k, 0) < prev:
            self.seen[q][k] = prev
            waits.append((k, prev))
        self._emit_waits(q, waits)
        self.cnt[k] += 16
        n = self.cnt[k]
        h = self.semh[k]
        self.prog[q].append(lambda e, fn=fn, h=h: fn(e).then_inc(h, 16))
        self.n_inst += 1
        for b in writes:
            b.w = (k, n)
            b.r = {}
        for b in reads:
            if b.r.get(k, 0) < n:
                b.r[k] = n

    def finish(self, bufs):
        need = {}
        for b in bufs:
            if b.w is not None and need.get(b.w[0], 0) < b.w[1]:
                need[b.w[0]] = b.w[1]
            for k, v in b.r.items():
                if need.get(k, 0) < v:
                    need[k] = v
        self._emit_waits("sp", list(need.items()))

    def emit(self):
        nc = self.nc
        with nc.Block() as block:
            @block.tensor
            def _(e):
                for f in self.prog["pe"]:
                    f(e)

            @block.scalar
            def _(e):
                for f in self.prog["act"]:
                    f(e)

            @block.vector
            def _(e):
                for f in self.prog["dve"]:
                    f(e)

            @block.gpsimd
            def _(e):
                for f in self.prog["pool"]:
                    f(e)

            @block.sync
            def _(e):
                for f in self.prog["sp"]:
                    f(e)


def TT(out, in0, in1, op):
    return lambda e: e.tensor_tensor(out=out, in0=in0, in1=in1, op=op)


def TS(out, in0, s1, s2, op0, op1=None):
    if op1 is None:
        return lambda e: e.tensor_scalar(out=out, in0=in0, scalar1=s1, scalar2=None, op0=op0)
    return lambda e: e.tensor_scalar(out=out, in0=in0, scalar1=s1, scalar2=s2, op0=op0, op1=op1)


def STT(out, in0, scalar, in1, op0, op1, accum_out=None):
    if accum_out is None:
        return lambda e: e.scalar_tensor_tensor(out=out, in0=in0, scalar=scalar, in1=in1, op0=op0, op1=op1)
    return lambda e: e.scalar_tensor_tensor(out=out, in0=in0, scalar=scalar, in1=in1, op0=op0, op1=op1,
                                            accum_out=accum_out)


def ACTF(out, in_, func, bias=None, scale=None, accum_out=None):
    kw = {}
    if bias is not None:
        kw["bias"] = bias
    if scale is not None:
        kw["scale"] = scale
    if accum_out is not None:
        kw["accum_out"] = accum_out
    return lambda e: e.activation(out=out, in_=in_, func=func, **kw)


def CP(out, in_):
    return lambda e: e.tensor_copy(out=out, in_=in_)


def MM(out, lhsT, rhs, start, stop):
    return lambda e: e.matmul(out=out, lhsT=lhsT, rhs=rhs, start=start, stop=stop)


def TR(out, in_, ident):
    return lambda e: e.transpose(out=out, in_=in_, identity=ident)


def RED(out, in_, op, axis=AX.X):
    return lambda e: e.tensor_reduce(out=out, in_=in_, axis=axis, op=op)


def DMA(out, in_):
    return lambda e: e.dma_start(out=out, in_=in_)


def GATHER(out, table, idx_col):
    return lambda e: e.indirect_dma_start(
        out=out, out_offset=None, in_=table,
        in_offset=bass.IndirectOffsetOnAxis(ap=idx_col, axis=0))


class T:
    def __init__(self, nc, name, shape, dtype, psum=False):
        if psum:
            self.t = nc.alloc_psum_tensor(name, shape, dtype)
        else:
            self.t = nc.alloc_sbuf_tensor(name, shape, dtype)
        self.b = Buf(name)

    def __getitem__(self, key):
        return self.t[key]


def build(NSEQ, S, debug=False):
    NB = S // 128
    nc = bass.Bass("TRN2", target_bir_lowering=False)

    def din(name, shape):
        return nc.dram_tensor(name, shape, F32, kind="ExternalInput").ap()

    x = din("x", [NSEQ, S, D])
    mix_norm_g = din("mix_norm_g", [1, D])
    w_in = din("w_in", [1, D, 3584])
    pool_w = din("pool_w", [1, 4, 128, 128])
    pool_scale = din("pool_scale", [1, 512])
    w_branch_a = din("w_branch_a", [1, 512, D])
    conv_w = din("conv_w", [1, 31, 512])
    conv_b = din("conv_b", [1, 512])
    conv_ln_g = din("conv_ln_g", [1, 512])
    conv_ln_b = din("conv_ln_b", [1, 512])
    w_branch_b = din("w_branch_b", [1, 512, D])
    gate_b = din("gate_b", [1, 2, D])
    w_out = din("w_out", [1, D, D])
    ffn_norm_g = din("ffn_norm_g", [1, D])
    peer_w_q = din("peer_w_q", [1, D, 2048])
    peer_sub_keys = din("peer_sub_keys", [1, 8, 2, 128, 128])
    peer_u = din("peer_u", [1, 16384, D])
    peer_v = din("peer_v", [1, 16384, D])
    final_norm_g = din("final_norm_g", [D])
    y = nc.dram_tensor("y", [NSEQ, S, D], F32, kind="ExternalOutput").ap()

    S_ = Sync(nc)
    defer = [None]

    def op(eng, fn, reads=(), writes=()):
        if defer[0] is not None:
            defer[0].append((S_.op, eng, fn, tuple(reads), tuple(writes)))
        else:
            S_.op(eng, fn, reads, writes)

    def dma(q, fn, reads=(), writes=()):
        if defer[0] is not None:
            defer[0].append((S_.dma, q, fn, tuple(reads), tuple(writes)))
        else:
            S_.dma(q, fn, reads, writes)

    def sb(name, shape, dtype=F32):
        return T(nc, name, shape, dtype)

    poolw = sb("poolw", [128, 4, 128], BF16)
    skT = sb("skT", [128, 16, 128], BF16)
    NWG = 4
    NWA = 2
    WG = [sb("WG%d" % i, [128, 8, 512], BF16) for i in range(NWG)]
    wg_rr = [0]

    ident_f = sb("ident_f", [128, 128])
    ident_bf = sb("ident_bf", [128, 128], BF16)
    ones_f = sb("ones_f", [128, 128])
    iof_i = sb("iof_i", [128, 128], I32)
    iop_i = sb("iop_i", [128, 1], I32)
    iof = sb("iof", [128, 128])
    iop = sb("iop", [128, 1])
    rcfix = sb("rcfix", [128, 4, 15])
    rc_i = sb("rc_i", [128, 4, 15], I32)
    T1 = sb("T1", [128, 128])
    T2 = sb("T2", [128, 128])
    cwT = sb("cwT", [128, 124])
    vT = sb("vT", [128, 40])
    g2bc = sb("g2bc", [128, D])
    gfbc = sb("gfbc", [128, D])
    sknat = [sb("sknat%d" % i, [128, 128], BF16) for i in range(2)]
    eps_t = sb("eps_t", [128, 1])

    PT = T(nc, "PT", [128, 1024], BF16, psum=True)
    PF = nc.alloc_psum_tensor("PF", [128, 7, 512], F32)
    bPF = [Buf("PF%d" % i) for i in range(7)]

    def pf2(i):
        return PF[:, i:i + 2, :].rearrange("p b f -> p (b f)")

    op("pool", lambda e: e.iota(out=iof_i[:], pattern=[[1, 128]], base=0, channel_multiplier=0), writes=[iof_i.b])
    op("pool", lambda e: e.iota(out=iop_i[:], pattern=[[0, 1]], base=0, channel_multiplier=1), writes=[iop_i.b])
    op("pool", lambda e: e.iota(out=rc_i[:], pattern=[[0, 4], [1, 15]], base=1, channel_multiplier=0), writes=[rc_i.b])
    op("dve", CP(iof[:], iof_i[:]), reads=[iof_i.b], writes=[iof.b])
    op("dve", CP(iop[:], iop_i[:]), reads=[iop_i.b], writes=[iop.b])
    op("dve", TS(ident_f[:], iof[:], iop[:, 0:1], None, ALU.is_equal), reads=[iof.b, iop.b], writes=[ident_f.b])
    op("dve", CP(ident_bf[:], ident_f[:]), reads=[ident_f.b], writes=[ident_bf.b])
    op("dve", lambda e: e.memset(ones_f[:], 1.0 / 512.0), writes=[ones_f.b])
    op("dve", lambda e: e.memset(eps_t[:], EPS), writes=[eps_t.b])
    op("dve", CP(rcfix[:], rc_i[:]), reads=[rc_i.b], writes=[rcfix.b])
    for g in range(4):
        w = float(2 ** (g + 1))
        op("dve", TS(rcfix[:, g, :], rcfix[:, g, :], w, None, ALU.min), reads=[rcfix.b], writes=[rcfix.b])
    op("dve", lambda e: e.reciprocal(out=rcfix[:], in_=rcfix[:]), reads=[rcfix.b], writes=[rcfix.b])

    op("dve", lambda e: e.memset(T1[:], 0.0), writes=[T1.b])
    op("dve", lambda e: e.memset(T2[:], 0.0), writes=[T2.b])
    dma("sp", DMA(T1[0:124, :], conv_w[0].rearrange("k (c p) -> (k c) p", p=128)), writes=[T1.b])
    row = 0
    for src, n in ((mix_norm_g[0], 8), (pool_scale[0], 4), (conv_b[0], 4), (conv_ln_g[0], 4), (conv_ln_b[0], 4),
                   (gate_b[0, 0], 8), (gate_b[0, 1], 8)):
        dma("sp", DMA(T2[row:row + n, :], src.rearrange("(c p) -> c p", p=128)), writes=[T2.b])
        row += n
    op("pe", TR(PF[:, 0, 0:128], T1[:], ident_f[:]), reads=[T1.b, ident_f.b], writes=[bPF[0]])
    op("pe", TR(PF[:, 0, 128:256], T2[:], ident_f[:]), reads=[T2.b, ident_f.b], writes=[bPF[0]])
    op("dve", CP(cwT[:], PF[:, 0, 0:124]), reads=[bPF[0]], writes=[cwT.b])
    op("dve", CP(vT[:], PF[:, 0, 128:168]), reads=[bPF[0]], writes=[vT.b])
    g1T, pscT, cbT, lgT, lbT, gbT = vT[:, 0:8], vT[:, 8:12], vT[:, 12:16], vT[:, 16:20], vT[:, 20:24], vT[:, 24:40]

    dma("sp", DMA(g2bc[:], ffn_norm_g[0].partition_broadcast(128)), writes=[g2bc.b])
    dma("sp", DMA(gfbc[:], final_norm_g.partition_broadcast(128)), writes=[gfbc.b])

    for c in range(4):
        dma("pool", DMA(poolw[:, c, :], pool_w[0, c]), writes=[poolw.b])
    for m in range(16):
        sn = sknat[m % 2]
        dma("pool", DMA(sn[:], peer_sub_keys[0, m // 2, m % 2]), writes=[sn.b])
        op("pe", TR(PT[:, 0:128], sn[:], ident_bf[:]), reads=[sn.b, ident_bf.b], writes=[PT.b])
        op("dve", CP(skT[:, m, :], PT[:, 0:128]), reads=[PT.b], writes=[skT.b])

    NG = 10
    gb = [sb("gb%d" % i, [128, 2 * D], BF16) for i in range(NG)]
    uv = nc.dram_tensor("uv_scr", [16384, 2 * D], BF16, kind="Internal").ap()
    win_bf = nc.dram_tensor("win_scr", [D, 3584], BF16, kind="Internal").ap()
    wq_bf = nc.dram_tensor("wq_scr", [D, 2048], BF16, kind="Internal").ap()
    b_uv = [Buf("uv%d" % j) for j in range(128)]
    b_win = [Buf("win%d" % j) for j in range(7)]
    b_wq = [Buf("wq%d" % j) for j in range(4)]
    wout_bf = nc.dram_tensor("wout_scr", [D, D], BF16, kind="Internal").ap()
    wab_bf = nc.dram_tensor("wab_scr", [2, 512, D], BF16, kind="Internal").ap()
    b_wout = [Buf("wout%d" % j) for j in range(2)]
    b_wab = [Buf("wab%d" % j) for j in range(2)]

    def wg4(w):
        return w[:].rearrange("p c f -> p (c f)").rearrange("p (g d) -> p g d", g=4)

    for j in range(7):
        dma("pool", DMA(win_bf[:, j * 512:(j + 1) * 512], w_in[0][:, j * 512:(j + 1) * 512]), writes=[b_win[j]])
    for j, src in enumerate((w_branch_a, w_branch_b)):
        dma("pool", DMA(wab_bf[j], src[0]), writes=[b_wab[j]])
    for j in range(2):
        dma("pool", DMA(wout_bf[:, j * 512:(j + 1) * 512], w_out[0][:, j * 512:(j + 1) * 512]), writes=[b_wout[j]])
    for j in range(4):
        dma("pool", DMA(wq_bf[:, j * 512:(j + 1) * 512], peer_w_q[0][:, j * 512:(j + 1) * 512]), writes=[b_wq[j]])
    for j in range(16):
        r0, r1 = j * 1024, (j + 1) * 1024
        dma("pool", DMA(uv[r0:r1, 0:D], peer_u[0, r0:r1, :]), writes=[b_uv[2 * j]])
        dma("pool", DMA(uv[r0:r1, D:2 * D], peer_v[0, r0:r1, :]), writes=[b_uv[2 * j + 1]])

    xt = [sb("xt%d" % i, [128, D]) for i in range(3)]
    junk_a = sb("junk_a", [128, D], BF16)
    junk_d = sb("junk_d", [128, D], BF16)
    stA = sb("stA", [128, 1])
    stC = sb("stC", [128, 1])
    stF = sb("stF", [128, 1])
    xn = sb("xn", [128, D], BF16)
    hT = sb("hT", [128, 8, 128], BF16)
    zap = sb("zap", [128, 4, 143])
    sA = sb("sA", [128, 4, 143])
    sB = sb("sB", [128, 4, 143])
    pooled = sb("pooled", [128, 4, 128], BF16)
    ptmp = sb("ptmp", [128, 4, 15])
    paT = sb("paT", [128, 4, 128], BF16)
    tA = sb("tA", [128, 4, 128])
    ubp = sb("ubp", [128, 4, 158], BF16)
    dgw = sb("dgw", [128, 124, 128], BF16)
    acc = sb("acc", [128, 4, 128])
    lnm = sb("lnm", [128, 4, 128])
    ub2 = sb("ub2", [128, 4, 128], BF16)
    gtA = sb("gtA", [128, 8, 128])
    gtB = sb("gtB", [128, 8, 128])
    merged = sb("merged", [128, 8, 128], BF16)
    hb2 = [sb("hb%d" % i, [128, D], BF16) for i in range(2)]
    hbT = sb("hbT", [128, 8, 128], BF16)
    qT = sb("qT", [128, 16, 128], BF16)
    s2 = [sb("s2_%d" % i, [128, 256]) for i in range(2)]
    vals = sb("vals", [128, 16, 16])
    idxu = sb("idxu", [128, 16, 16], U32)
    idxf = sb("idxf", [128, 16, 16])
    cand = sb("cand", [128, 8, 256])
    best = sb("best", [128, 8, 16])
    posu = sb("posu", [128, 8, 16], U32)
    ku = sb("ku", [128, 2, 128], U32)
    kf = sb("kf", [128, 2, 128])
    oh = sb("oh", [128, 8, 256], BF16)
    isel = sb("isel", [128, 2, 128])
    eidf = sb("eidf", [128, 128])
    eidi2 = [sb("eidi%d" % i, [128, 128], I32) for i in range(2)]
    gte = sb("gte", [128, 8, 16])
    gsum = sb("gsum", [128, 8])
    gate2 = [sb("gate%d" % i, [128, 128]) for i in range(2)]
    actv = sb("actv", [128, 128])
    gl = sb("gl", [128, 128])
    coef = sb("coef", [128, 128])
    GS = 2
    NGRP = 128 // GS
    b_act = [Buf("act%d" % i) for i in range(128)]
    b_gl = [Buf("gl%d" % i) for i in range(NGRP)]
    b_coef = [Buf("coef%d" % i) for i in range(NGRP)]
    dg = [sb("dg%d" % i, [128, 128], BF16) for i in range(6)]
    print("SBUF bytes remaining/partition:", nc.sbuf_bytes_remaining)

    op("dve", lambda e: e.memset(zap[:], 0.0), writes=[zap.b])
    op("dve", lambda e: e.memset(ubp[:], 0.0), writes=[ubp.b])
    op("dve", TT(dgw[:], ident_f[:].unsqueeze(1).to_broadcast([128, 124, 128]),
                 cwT[:].unsqueeze(2).to_broadcast([128, 124, 128]), ALU.mult), reads=[ident_f.b, cwT.b], writes=[dgw.b])

    blocks = [(s, i) for s in range(NSEQ) for i in range(NB)]

    def load_x(bi):
        if bi >= len(blocks):
            return
        s, i = blocks[bi]
        t = xt[bi % 3]
        dma("sp", DMA(t[:], x[s, i * 128:(i + 1) * 128, :]), writes=[t.b])

    def rstd_from_ss(st):
        c = st[:, 0:1]
        op("act", ACTF(c, c, AF.Ln, bias=eps_t[:, 0:1], scale=1.0 / D), reads=[st.b, eps_t.b], writes=[st.b])
        op("act", ACTF(c, c, AF.Exp, scale=-0.5), reads=[st.b], writes=[st.b])

    ringA = [0]
    ringC = [0]

    def ring_next(chain):
        if chain == "A":
            w = WG[ringA[0] % NWA]
            ringA[0] += 1
        else:
            w = WG[NWA + ringC[0] % (NWG - NWA)]
            ringC[0] += 1
        return w

    def load_wg(src, sbuf, col0, chain="A"):
        w = ring_next(chain)
        dma("sp", DMA(w[:], src[:, col0:col0 + 512].rearrange("(c p) f -> p c f", p=128)), reads=[sbuf[col0 // 512]],
            writes=[w.b])
        return w

    def load_wab(j):
        w = ring_next("A")
        dma("sp", DMA(wg4(w), wab_bf[j].rearrange("(g p) d -> p g d", p=128)), reads=[b_wab[j]], writes=[w.b])
        return w

    def proj4(w, rhsT, bank, rhs_b):
        for j in range(4):
            for c in range(8):
                op("pe", MM(PF[:, bank, j * 128:(j + 1) * 128], w[:, c, j * 128:(j + 1) * 128], rhsT[:, c, :],
                            c == 0, c == 7), reads=[w.b, rhs_b], writes=[bPF[bank]])

    PTA = PF[:, 1, :].bitcast(BF16)
    PCF = PT[:].bitcast(F32)

    def proAB(bi):
        s, i = blocks[bi]
        first = (i == 0)
        X = xt[bi % 3]

        op("act", ACTF(junk_a[:], X[:], AF.Square, accum_out=stA[:, 0:1]), reads=[X.b], writes=[stA.b, junk_a.b])
        rstd_from_ss(stA)
        op("act", ACTF(xn[:], X[:], AF.Copy, scale=stA[:, 0:1]), reads=[X.b, stA.b], writes=[xn.b])
        yield
        for c in range(8):
            op("pe", TR(PTA[:, c * 128:(c + 1) * 128], xn[:, c * 128:(c + 1) * 128], ident_bf[:]),
               reads=[xn.b, ident_bf.b], writes=[bPF[1]])
        op("dve", TT(hT[:], PTA.rearrange("p (c t) -> p c t", c=8), g1T.unsqueeze(2).to_broadcast([128, 8, 128]),
                     ALU.mult), reads=[bPF[1], vT.b], writes=[hT.b])
        yield

        w = load_wg(win_bf, b_win, 0)
        proj4(w, hT, 0, hT.b)
        yield
        op("act", ACTF(zap[:, :, 15:143], PF[:, 0, :].rearrange("p (g t) -> p g t", g=4), AF.Copy),
           reads=[bPF[0]], writes=[zap.b])
        op("dve", TT(sA[:, :, 1:143], zap[:, :, 1:143], zap[:, :, 0:142], ALU.add), reads=[zap.b], writes=[sA.b])
        op("dve", TT(sB[:, 1:4, 3:143], sA[:, 1:4, 3:143], sA[:, 1:4, 1:141], ALU.add), reads=[sA.b], writes=[sB.b])
        yield
        op("dve", TT(sA[:, 2:4, 7:143], sB[:, 2:4, 7:143], sB[:, 2:4, 3:139], ALU.add), reads=[sB.b], writes=[sA.b])
        op("dve", TT(sB[:, 3:4, 15:143], sA[:, 3:4, 15:143], sA[:, 3:4, 7:135], ALU.add), reads=[sA.b], writes=[sB.b])
        yield
        srcs = (sA, sB, sA, sB)
        for g in range(4):
            wdw = float(2 ** (g + 1))
            sg = srcs[g]
            op("dve", STT(pooled[:, g, :], sg[:, g, 15:143], 1.0 / wdw, zap[:, g, 15:143], ALU.mult, ALU.subtract),
               reads=[sg.b, zap.b], writes=[pooled.b])
        yield
        if first:
            for g in range(4):
                sg = srcs[g]
                op("dve", TT(ptmp[:, g, :], sg[:, g, 15:30], rcfix[:, g, :], ALU.mult), reads=[sg.b, rcfix.b],
                   writes=[ptmp.b])
            op("dve", TT(pooled[:, :, 0:15], ptmp[:], zap[:, :, 15:30], ALU.subtract), reads=[ptmp.b, zap.b],
               writes=[pooled.b])
            yield
        if i + 1 < NB:
            op("act", ACTF(zap[:, :, 0:15], zap[:, :, 128:143], AF.Copy), reads=[zap.b], writes=[zap.b])
        else:
            op("dve", lambda e: e.memset(zap[:, :, 0:15], 0.0), writes=[zap.b])
        for g in range(4):
            op("pe", MM(PF[:, 0, g * 128:(g + 1) * 128], poolw[:, g, :], pooled[:, g, :], True, True),
               reads=[poolw.b, pooled.b], writes=[bPF[0]])
        op("dve", TT(paT[:], PF[:, 0, :].rearrange("p (g t) -> p g t", g=4),
                     pscT.unsqueeze(2).to_broadcast([128, 4, 128]), ALU.mult), reads=[bPF[0], vT.b], writes=[paT.b])
        yield
        YA = pf2(3)
        wA = load_wab(0)
        Wa = wg4(wA)
        for dc in range(8):
            for g in range(4):
                op("pe", MM(YA[:, dc * 128:(dc + 1) * 128], Wa[:, g, dc * 128:(dc + 1) * 128], paT[:, g, :],
                            g == 0, g == 3), reads=[wA.b, paT.b], writes=[bPF[3], bPF[4]])
            if dc % 4 == 3:
                yield
        for half in range(2):
            w = load_wg(win_bf, b_win, 1536 + half * 512)
            proj4(w, hT, half, hT.b)
            yield
            for j in range(4):
                dc = half * 4 + j
                op("act", ACTF(gtA[:, dc, :], PF[:, half, j * 128:(j + 1) * 128], AF.Sigmoid,
                               bias=gbT[:, dc:dc + 1]), reads=[bPF[half], vT.b], writes=[gtA.b])
            yield
        op("dve", TT(gtA[:].rearrange("p c t -> p (c t)"), gtA[:].rearrange("p c t -> p (c t)"), YA, ALU.mult),
           reads=[gtA.b, bPF[3], bPF[4]], writes=[gtA.b])
        yield

        w = load_wg(win_bf, b_win, 512)
        proj4(w, hT, 1, hT.b)
        yield
        w = load_wg(win_bf, b_win, 1024)
        proj4(w, hT, 2, hT.b)
        yield
        op("act", ACTF(tA[:], PF[:, 2, :].rearrange("p (g t) -> p g t", g=4), AF.Sigmoid), reads=[bPF[2]],
           writes=[tA.b])
        op("dve", TT(ubp[:, :, 30:158], PF[:, 1, :].rearrange("p (g t) -> p g t", g=4), tA[:], ALU.mult),
           reads=[bPF[1], tA.b], writes=[ubp.b])
        yield
        for c in range(4):
            for k in range(31):
                op("pe", MM(PF[:, 2, c * 128:(c + 1) * 128], dgw[:, k * 4 + c, :], ubp[:, c, k:k + 128], k == 0, k == 30),
                   reads=[dgw.b, ubp.b], writes=[bPF[2]])
            yield
        op("dve", TT(acc[:], PF[:, 2, :].rearrange("p (g t) -> p g t", g=4),
                     cbT.unsqueeze(2).to_broadcast([128, 4, 128]), ALU.add), reads=[bPF[2], vT.b], writes=[acc.b])
        if i + 1 < NB:
            op("act", ACTF(ubp[:, :, 0:30], ubp[:, :, 128:158], AF.Copy), reads=[ubp.b], writes=[ubp.b])
        else:
            op("dve", lambda e: e.memset(ubp[:, :, 0:30], 0.0), writes=[ubp.b])
        op("act", ACTF(tA[:], acc[:], AF.Square), reads=[acc.b], writes=[tA.b])
        for c in range(4):
            op("pe", MM(PF[:, 1, 0:128], ones_f[:], acc[:, c, :], c == 0, c == 3), reads=[ones_f.b, acc.b],
               writes=[bPF[1]])
        for c in range(4):
            op("pe", MM(PF[:, 1, 128:256], ones_f[:], tA[:, c, :], c == 0, c == 3), reads=[ones_f.b, tA.b],
               writes=[bPF[1]])
        yield
        op("dve", CP(lnm[:, 0, :], PF[:, 1, 0:128]), reads=[bPF[1]], writes=[lnm.b])
        op("dve", TT(lnm[:, 1, :], lnm[:, 0, :], lnm[:, 0, :], ALU.mult), reads=[lnm.b], writes=[lnm.b])
        op("dve", TT(lnm[:, 2, :], PF[:, 1, 128:256], lnm[:, 1, :], ALU.subtract), reads=[bPF[1], lnm.b],
           writes=[lnm.b])
        op("act", ACTF(lnm[:, 2, :], lnm[:, 2, :], AF.Sqrt, bias=eps_t[:, 0:1]), reads=[lnm.b, eps_t.b],
           writes=[lnm.b])
        op("dve", lambda e: e.reciprocal(out=lnm[:, 3, :], in_=lnm[:, 2, :]), reads=[lnm.b], writes=[lnm.b])
        yield
        op("dve", TT(acc[:], acc[:], lnm[:, 0:1, :].to_broadcast([128, 4, 128]), ALU.subtract),
           reads=[acc.b, lnm.b], writes=[acc.b])
        op("dve", TT(acc[:], acc[:], lnm[:, 3:4, :].to_broadcast([128, 4, 128]), ALU.mult),
           reads=[acc.b, lnm.b], writes=[acc.b])
        yield
        op("dve", TT(acc[:], acc[:], lgT.unsqueeze(2).to_broadcast([128, 4, 128]), ALU.mult),
           reads=[acc.b, vT.b], writes=[acc.b])
        op("dve", TT(acc[:], acc[:], lbT.unsqueeze(2).to_broadcast([128, 4, 128]), ALU.add),
           reads=[acc.b, vT.b], writes=[acc.b])
        op("act", ACTF(tA[:], acc[:], AF.Sigmoid), reads=[acc.b], writes=[tA.b])
        op("dve", TT(ub2[:], acc[:], tA[:], ALU.mult), reads=[acc.b, tA.b], writes=[ub2.b])
        yield
        YB = pf2(3)
        wB = load_wab(1)
        Wb = wg4(wB)
        for dc in range(8):
            for c in range(4):
                op("pe", MM(YB[:, dc * 128:(dc + 1) * 128], Wb[:, c, dc * 128:(dc + 1) * 128], ub2[:, c, :],
                            c == 0, c == 3), reads=[wB.b, ub2.b], writes=[bPF[3], bPF[4]])
            if dc % 4 == 3:
                yield
        for half in range(2):
            w = load_wg(win_bf, b_win, 2560 + half * 512)
            proj4(w, hT, half, hT.b)
            yield
            for j in range(4):
                dc = half * 4 + j
                op("act", ACTF(gtB[:, dc, :], PF[:, half, j * 128:(j + 1) * 128], AF.Sigmoid,
                               bias=gbT[:, 8 + dc:8 + dc + 1]), reads=[bPF[half], vT.b], writes=[gtB.b])
            yield
        op("dve", TT(gtB[:].rearrange("p c t -> p (c t)"), gtB[:].rearrange("p c t -> p (c t)"), YB, ALU.mult),
           reads=[gtB.b, bPF[3], bPF[4]], writes=[gtB.b])
        op("dve", TT(merged[:], gtA[:], gtB[:], ALU.add), reads=[gtA.b, gtB.b], writes=[merged.b])
        yield
        PO = pf2(3)
        for half in range(2):
            wO = load_wg(wout_bf, b_wout, half * 512)
            for c in range(8):
                op("pe", MM(PO[:, half * 512:(half + 1) * 512], merged[:, c, :], wO[:, c, :],
                            c == 0, c == 7), reads=[merged.b, wO.b], writes=[bPF[3 + half]])
            yield
        op("dve", TT(X[:], X[:], PO, ALU.add), reads=[X.b, bPF[3], bPF[4]], writes=[X.b])
        yield

    def proC(bi):
        X = xt[bi % 3]
        hb = hb2[bi % 2]
        eidi = eidi2[bi % 2]
        gate = gate2[bi % 2]
        op("act", ACTF(junk_a[:], X[:], AF.Square, accum_out=stC[:, 0:1]), reads=[X.b], writes=[stC.b, junk_a.b])
        rstd_from_ss(stC)
        op("dve", STT(hb[:], X[:], stC[:, 0:1], g2bc[:], ALU.mult, ALU.mult), reads=[X.b, stC.b, g2bc.b],
           writes=[hb.b])
        yield
        for c in range(8):
            op("pe", TR(PT[:, c * 128:(c + 1) * 128], hb[:, c * 128:(c + 1) * 128], ident_bf[:]),
               reads=[hb.b, ident_bf.b], writes=[PT.b])
        op("act", ACTF(hbT[:], PT[:].rearrange("p (c t) -> p c t", c=8), AF.Copy), reads=[PT.b], writes=[hbT.b])
        yield
        for mg in range(4):
            w = load_wg(wq_bf, b_wq, mg * 512, chain="C")
            for j in range(4):
                for c in range(8):
                    op("pe", MM(PCF[:, j * 128:(j + 1) * 128], w[:, c, j * 128:(j + 1) * 128], hbT[:, c, :],
                                c == 0, c == 7), reads=[w.b, hbT.b], writes=[PT.b])
            op("act", ACTF(qT[:, mg * 4:(mg + 1) * 4, :], PCF.rearrange("p (j t) -> p j t", j=4), AF.Copy),
               reads=[PT.b], writes=[qT.b])
            yield
        for mg in range(4):
            for j in range(4):
                m = mg * 4 + j
                op("pe", MM(PCF[:, j * 128:(j + 1) * 128], qT[:, m, :], skT[:, m, :], True, True),
                   reads=[qT.b, skT.b], writes=[PT.b])
            for j in range(4):
                m = mg * 4 + j
                sv = PCF[:, j * 128:(j + 1) * 128]
                bb = PT.b
                s2t = s2[m % 2]
                op("dve", lambda e, sv=sv, m=m: e.max(out=vals[:, m, 0:8], in_=sv), reads=[bb], writes=[vals.b])
                op("dve", lambda e, sv=sv, m=m: e.max_index(out=idxu[:, m, 0:8], in_max=vals[:, m, 0:8], in_values=sv),
                   reads=[bb, vals.b], writes=[idxu.b])
                op("dve", lambda e, sv=sv, m=m, s2t=s2t: e.match_replace(out=s2t[:, 0:128],
                                                                       in_to_replace=vals[:, m, 0:8],
                                                                       in_values=sv, imm_value=NEG),
                   reads=[bb, vals.b], writes=[s2t.b])
                op("dve", lambda e, m=m, s2t=s2t: e.max(out=vals[:, m, 8:16], in_=s2t[:, 0:128]), reads=[s2t.b],
                   writes=[vals.b])
                op("dve", lambda e, m=m, s2t=s2t: e.max_index(out=idxu[:, m, 8:16], in_max=vals[:, m, 8:16],
                                                              in_values=s2t[:, 0:128]),
                   reads=[s2t.b, vals.b], writes=[idxu.b])
            yield
        op("dve", CP(idxf[:], idxu[:]), reads=[idxu.b], writes=[idxf.b])
        v4 = vals[:].rearrange("p (h two) k -> p h two k", two=2)
        i4 = idxf[:].rearrange("p (h two) k -> p h two k", two=2)
        c4 = cand[:].rearrange("p h (a b) -> p h a b", a=16)
        op("dve", TT(c4, v4[:, :, 0, :].unsqueeze(3).to_broadcast([128, 8, 16, 16]),
                     v4[:, :, 1, :].unsqueeze(2).to_broadcast([128, 8, 16, 16]), ALU.add),
           reads=[vals.b], writes=[cand.b])
        yield
        for h in range(8):
            s2t = s2[h % 2]
            ch = cand[:, h, :]
            op("dve", lambda e, ch=ch, h=h: e.max(out=best[:, h, 0:8], in_=ch), reads=[cand.b], writes=[best.b])
            op("dve", lambda e, ch=ch, h=h: e.max_index(out=posu[:, h, 0:8], in_max=best[:, h, 0:8], in_values=ch),
               reads=[cand.b, best.b], writes=[posu.b])
            op("dve", lambda e, ch=ch, h=h, s2t=s2t: e.match_replace(out=s2t[:], in_to_replace=best[:, h, 0:8],
                                                                   in_values=ch, imm_value=NEG),
               reads=[cand.b, best.b], writes=[s2t.b])
            op("dve", lambda e, h=h, s2t=s2t: e.max(out=best[:, h, 8:16], in_=s2t[:]), reads=[s2t.b], writes=[best.b])
            op("dve", lambda e, h=h, s2t=s2t: e.max_index(out=posu[:, h, 8:16], in_max=best[:, h, 8:16],
                                                          in_values=s2t[:]),
               reads=[s2t.b, best.b], writes=[posu.b])
            yield
        pu = posu[:].rearrange("p h k -> p (h k)")
        op("dve", lambda e: e.tensor_single_scalar(out=ku[:, 0, :], in_=pu, scalar=4, op=ALU.logical_shift_right),
           reads=[posu.b], writes=[ku.b])
        op("dve", lambda e: e.tensor_single_scalar(out=ku[:, 1, :], in_=pu, scalar=15, op=ALU.bitwise_and),
           reads=[posu.b], writes=[ku.b])
        op("dve", CP(kf[:], ku[:]), reads=[ku.b], writes=[kf.b])
        yield
        o4 = oh[:].rearrange("p h (a b) -> p h a b", a=16)
        for half in range(2):
            k3 = kf[:, half, :].rearrange("p (h k) -> p h k", h=8)
            op("dve", TT(o4, k3.unsqueeze(3).to_broadcast([128, 8, 16, 16]),
                         iof[:, 0:16].unsqueeze(1).unsqueeze(1).to_broadcast([128, 8, 16, 16]), ALU.is_equal),
               reads=[kf.b, iof.b], writes=[oh.b])
            yield
            op("dve", TT(o4, o4, i4[:, :, half, :].unsqueeze(2).to_broadcast([128, 8, 16, 16]), ALU.mult),
               reads=[oh.b, idxf.b], writes=[oh.b])
            yield
            op("dve", RED(isel[:, half, :], oh[:].rearrange("p h (a b) -> p (h a) b", a=16), ALU.add),
               reads=[oh.b], writes=[isel.b])
            yield
        op("dve", STT(eidf[:], isel[:, 0, :], 128.0, isel[:, 1, :], ALU.mult, ALU.add), reads=[isel.b], writes=[eidf.b])
        op("dve", CP(eidi[:], eidf[:]), reads=[eidf.b], writes=[eidi.b])
        op("dve", TT(gte[:], best[:], best[:, :, 0:1].to_broadcast([128, 8, 16]), ALU.subtract), reads=[best.b],
           writes=[gte.b])
        op("act", ACTF(gte[:], gte[:], AF.Exp), reads=[gte.b], writes=[gte.b])
        yield
        op("dve", RED(gsum[:], gte[:], ALU.add), reads=[gte.b], writes=[gsum.b])
        op("dve", lambda e: e.reciprocal(out=gsum[:], in_=gsum[:]), reads=[gsum.b], writes=[gsum.b])
        op("dve", TT(gate[:].rearrange("p (h k) -> p h k", h=8), gte[:], gsum[:].unsqueeze(2).to_broadcast([128, 8, 16]),
                     ALU.mult), reads=[gte.b, gsum.b], writes=[gate.b])
        yield

    gbrr = [0]

    def main(bi, thunks):
        s, i = blocks[bi]
        X = xt[bi % 3]
        hb = hb2[bi % 2]
        eidi = eidi2[bi % 2]
        gate = gate2[bi % 2]
        PA = pf2(5)
        pos = [0]
        per = -(-len(thunks) // 112) if thunks else 0

        def advance(n):
            for _ in range(n):
                if pos[0] >= len(thunks):
                    return
                f, a, fn, r, w = thunks[pos[0]]
                pos[0] += 1
                f(a, fn, r, w)

        for grp in range(NGRP):
            sl0 = grp * GS
            bufs = []
            for sl in range(sl0, sl0 + GS):
                g = gb[gbrr[0] % NG]
                gbrr[0] += 1
                bufs.append(g)
                dma("pool", GATHER(g[:], uv, eidi[:, sl:sl + 1]), reads=[eidi.b] + (b_uv if bi == 0 and sl == 0 else []),
                    writes=[g.b])
                op("dve", STT(junk_d[:], g[:, 0:D], 1.0, hb[:], ALU.mult, ALU.mult, accum_out=actv[:, sl:sl + 1]),
                   reads=[g.b, hb.b], writes=[b_act[sl], junk_d.b])
                advance(per)
            op("act", ACTF(gl[:, sl0:sl0 + GS], actv[:, sl0:sl0 + GS], AF.Gelu_apprx_tanh), reads=b_act[sl0:sl0 + GS],
               writes=[b_gl[grp]])
            for k, sl in enumerate(range(sl0, sl0 + GS)):
                g = bufs[k]
                dgt = dg[sl % 6]
                op("act", ACTF(coef[:, sl:sl + 1], gl[:, sl:sl + 1], AF.Copy, scale=gate[:, sl:sl + 1]),
                   reads=[b_gl[grp], gate.b], writes=[b_coef[grp]])
                op("act", ACTF(dgt[:], ident_bf[:], AF.Copy, scale=coef[:, sl:sl + 1]), reads=[ident_bf.b, b_coef[grp]],
                   writes=[dgt.b])
                for half in range(2):
                    op("pe", MM(PA[:, half * 512:(half + 1) * 512], dgt[:], g[:, D + half * 512:D + (half + 1) * 512],
                                sl == 0, sl == 127), reads=[dgt.b, g.b], writes=[bPF[5 + half]])
        advance(len(thunks))
        op("dve", TT(X[:], X[:], PA, ALU.add), reads=[X.b, bPF[5], bPF[6]], writes=[X.b])
        op("act", ACTF(junk_a[:], X[:], AF.Square, accum_out=stF[:, 0:1]), reads=[X.b], writes=[stF.b, junk_a.b])
        rstd_from_ss(stF)
        op("dve", STT(X[:], X[:], stF[:, 0:1], gfbc[:], ALU.mult, ALU.mult), reads=[X.b, stF.b, gfbc.b], writes=[X.b])
        dma("sp", DMA(y[s, i * 128:(i + 1) * 128, :], X[:]), reads=[X.b])
        load_x(bi + 3)

    def collect(gen):
        lst = []
        defer[0] = lst
        for _ in gen:
            pass
        defer[0] = None
        return lst

    def merge(a, b):
        out = []
        ia = ib = 0
        while ia < len(a) or ib < len(b):
            if ib >= len(b) or (ia < len(a) and ia * len(b) <= ib * len(a)):
                out.append(a[ia])
                ia += 1
            else:
                out.append(b[ib])
                ib += 1
        return out

    nblk = len(blocks)
    load_x(0)
    load_x(1)
    load_x(2)
    for _ in proAB(0):
        pass
    fill = merge(collect(proC(0)), collect(proAB(1)) if nblk > 1 else [])
    for f, a, fn, r, w in fill:
        f(a, fn, r, w)
    for bi in range(nblk):
        tc = collect(proC(bi + 1)) if bi + 1 < nblk else []
        tab = collect(proAB(bi + 2)) if bi + 2 < nblk else []
        main(bi, merge(tc, tab))

    S_.finish([t.b for t in xt])
    S_.emit()
    return nc, S_


_CACHE = {}


def kernel(**inputs):
    x = np.ascontiguousarray(np.asarray(inputs["x"], dtype=np.float32))
    B, S, _ = x.shape
    nseq = B // NCORES
    key = (nseq, S)
    if key not in _CACHE:
        _CACHE[key] = build(nseq, S)[0]
    nc = _CACHE[key]
    shared = {k: np.ascontiguousarray(np.asarray(v, dtype=np.float32)) for k, v in inputs.items() if k != "x"}
    in_maps = []
    for c in range(NCORES):
        m = dict(shared)
        m["x"] = x[c * nseq:(c + 1) * nseq]
        in_maps.append(m)
    res = run_bass_kernel_spmd(nc, in_maps, core_ids=list(range(NCORES)))
    return np.concatenate([r["y"] for r in res.results], axis=0)
```
